# Optimizing a Trainium2 kernel written in Bass

```python
import math
import jax, jax.numpy as jnp
from jax import lax
import numpy as np

D_MODEL = 1024
BATCH = 4
SEQ = 4096
DEPTH = 2

PLE_DIM = 256
HEAD_DIM = 64
DIFF_HEADS = 4
DIFF_QK = 2 * HEAD_DIM
DIFF_V = 2 * HEAD_DIM
FOX_HEADS = 4
FOX_DIM = HEAD_DIM
RET_HEADS = 4
RET_DIM = HEAD_DIM
WA = DIFF_HEADS * DIFF_V
WB = FOX_HEADS * FOX_DIM
WC = RET_HEADS * RET_DIM
MIX_WIDTH = WA + WB + WC
IN_SIZES = (DIFF_HEADS * DIFF_QK, DIFF_HEADS * DIFF_QK, WA, WB, WB, WB, FOX_HEADS, WC, WC, WC, WC)
IN_COLS = sum(IN_SIZES)
Q_BLOCK = 128
RET_CHUNK = 128
RET_THETA = 10000.0
D_FF_DENSE = 2816
N_EXPERTS = 8
TOP_K = 2
D_FF_EXPERT = 3584
N_DENSE = (DEPTH + 1) // 2
N_MOE = DEPTH // 2
EPS = 1e-6
NEG_INF = -1e30

kernel_name = "hymba_diff_fox_retnet_moe_trunk"


def rmsnorm(x, g):
    xf = x.astype(jnp.float32)
    y = xf * lax.rsqrt(jnp.mean(xf * xf, axis=-1, keepdims=True) + EPS)
    return (y * g.astype(jnp.float32)).astype(x.dtype)


def head_groupnorm(x):
    xf = x.astype(jnp.float32)
    mu = jnp.mean(xf, axis=-1, keepdims=True)
    var = jnp.mean(jnp.square(xf - mu), axis=-1, keepdims=True)
    return ((xf - mu) * lax.rsqrt(var + EPS)).astype(x.dtype)


def split_heads(t, n_heads):
    B, S, _ = t.shape
    return t.reshape(B, S, n_heads, -1).transpose(0, 2, 1, 3)


def merge_heads(t):
    B, H, S, D = t.shape
    return t.transpose(0, 2, 1, 3).reshape(B, S, H * D)


def rotary(x):
    S, D = x.shape[2], x.shape[3]
    half = D // 2
    inv = RET_THETA ** (-jnp.arange(half, dtype=jnp.float32) / half)
    ang = jnp.arange(S, dtype=jnp.float32)[:, None] * inv[None, :]
    cos, sin = jnp.cos(ang).astype(x.dtype), jnp.sin(ang).astype(x.dtype)
    x1, x2 = x[..., :half], x[..., half:]
    return jnp.concatenate([x1 * cos - x2 * sin, x1 * sin + x2 * cos], axis=-1)


def causal_block_attention(q, k, v, scale, cum_logf=None):
    B, H, S, Dk = q.shape
    nb = S // Q_BLOCK
    q_blocks = q.reshape(B, H, nb, Q_BLOCK, Dk).transpose(2, 0, 1, 3, 4)
    k_pos = jnp.arange(S)
    xs = [jnp.arange(nb), q_blocks]
    if cum_logf is not None:
        xs.append(cum_logf.reshape(B, H, nb, Q_BLOCK).transpose(2, 0, 1, 3))

    def one_block(blk):
        i, q_i = blk[0], blk[1]
        s = jnp.einsum('bhqd,bhkd->bhqk', q_i, k).astype(jnp.float32) * scale
        if cum_logf is not None:
            s = s + blk[2][..., :, None] - cum_logf[:, :, None, :]
        q_pos = i * Q_BLOCK + jnp.arange(Q_BLOCK)
        s = jnp.where(k_pos[None, :] <= q_pos[:, None], s, NEG_INF)
        w = jax.nn.softmax(s, axis=-1).astype(v.dtype)
        return jnp.einsum('bhqk,bhkv->bhqv', w, v)

    o = lax.map(one_block, tuple(xs))
    return o.transpose(1, 2, 0, 3, 4).reshape(B, H, S, v.shape[-1])


def retention_chunkwise(q, k, v):
    B, H, S, Dk = q.shape
    Dv = v.shape[-1]
    nc = S // RET_CHUNK
    log_g = jnp.log1p(-jnp.exp2(-5.0 - jnp.arange(H, dtype=jnp.float32)))
    j = jnp.arange(RET_CHUNK, dtype=jnp.float32)
    diff = j[:, None] - j[None, :]
    inner = jnp.exp(jnp.where(diff[None] >= 0, diff[None] * log_g[:, None, None], -jnp.inf))
    q_dec = jnp.exp((j + 1.0)[None, :] * log_g[:, None])
    k_dec = jnp.exp((RET_CHUNK - 1.0 - j)[None, :] * log_g[:, None])
    c_dec = jnp.exp(RET_CHUNK * log_g)

    def to_chunks(t):
        return t.astype(jnp.float32).reshape(B, H, nc, RET_CHUNK, -1).transpose(2, 0, 1, 3, 4)

    def step(state, qkv):
        qc, kc, vc = qkv
        a = jnp.einsum('bhqd,bhkd->bhqk', qc, kc) * inner
        o = (jnp.einsum('bhqk,bhkv->bhqv', a, vc)
             + jnp.einsum('bhqd,bhdv->bhqv', qc * q_dec[:, :, None], state))
        state = state * c_dec[:, None, None] + jnp.einsum('bhkd,bhkv->bhdv', kc * k_dec[:, :, None], vc)
        return state, o

    state0 = jnp.zeros((B, H, Dk, Dv), jnp.float32)
    _, o = lax.scan(step, state0, (to_chunks(q), to_chunks(k), to_chunks(v)))
    return o.transpose(1, 2, 0, 3, 4).reshape(B, H, S, Dv).astype(v.dtype)


def hybrid_mixer(xn, w_in, b_forget, lq1, lk1, lq2, lk2, subln_g, ret_gn_g, w_out, layer_idx):
    B, S, _ = xn.shape
    proj = xn @ w_in
    qa, ka, va, qb, kb, vb, fb, qc, kc, vc, gc = jnp.split(proj, list(np.cumsum(IN_SIZES)[:-1]), axis=-1)

    qa = split_heads(qa, 2 * DIFF_HEADS)
    ka = split_heads(ka, 2 * DIFF_HEADS)
    va = jnp.repeat(split_heads(va, DIFF_HEADS), 2, axis=1)
    o = causal_block_attention(qa, ka, va, HEAD_DIM ** -0.5)
    o = o.reshape(B, DIFF_HEADS, 2, S, DIFF_V)
    lam_init = 0.8 - 0.6 * math.exp(-0.3 * layer_idx)
    lam = (jnp.exp(jnp.sum(lq1.astype(jnp.float32) * lk1.astype(jnp.float32)))
           - jnp.exp(jnp.sum(lq2.astype(jnp.float32) * lk2.astype(jnp.float32))) + lam_init)
    o_a = o[:, :, 0] - lam.astype(o.dtype) * o[:, :, 1]
    o_a = merge_heads(rmsnorm(o_a, subln_g) * (1.0 - lam_init))

    log_f = jax.nn.log_sigmoid((fb + b_forget).astype(jnp.float32))
    cum = jnp.cumsum(log_f, axis=1).transpose(0, 2, 1)
    o_b = causal_block_attention(split_heads(qb, FOX_HEADS), split_heads(kb, FOX_HEADS),
                                 split_heads(vb, FOX_HEADS), FOX_DIM ** -0.5, cum)
    o_b = merge_heads(o_b)

    qr = rotary(split_heads(qc, RET_HEADS))
    kr = rotary(split_heads(kc, RET_HEADS)) * (RET_DIM ** -0.5)
    o_c = retention_chunkwise(qr, kr, split_heads(vc, RET_HEADS))
    o_c = merge_heads(head_groupnorm(o_c)) * ret_gn_g
    o_c = jax.nn.silu(gc) * o_c

    return jnp.concatenate([o_a, o_b, o_c], axis=-1) @ w_out


def swiglu(x, wg, wu, wd):
    return (jax.nn.silu(x @ wg) * (x @ wu)) @ wd


def moe_swiglu(xn, router, wg, wu, wd):
    B, S, D = xn.shape
    t = xn.reshape(B * S, D)
    logits = (t @ router).astype(jnp.float32)
    top_val, top_idx = lax.top_k(logits, TOP_K)
    gates = jax.nn.softmax(top_val, axis=-1)
    combine = jnp.sum(jax.nn.one_hot(top_idx, N_EXPERTS, dtype=jnp.float32) * gates[..., None], axis=1)
    out = jnp.zeros_like(t)
    for e in range(N_EXPERTS):
        out = out + combine[:, e:e + 1].astype(t.dtype) * swiglu(t, wg[e], wu[e], wd[e])
    return out.reshape(B, S, D)


def setup_inputs(seed: int = 0) -> dict:
    key = jax.random.key(seed)
    ks = jax.random.split(key, 26)
    f32 = jnp.float32

    def nrm(k, shape, scale):
        return jax.random.normal(k, shape, f32) * scale

    def gain(k, shape):
        return 1.0 + 0.05 * jax.random.normal(k, shape, f32)

    return {
        "x": nrm(ks[0], (BATCH, SEQ, D_MODEL), 1.0),
        "p": nrm(ks[1], (DEPTH, BATCH, SEQ, PLE_DIM), 1.0),
        "norm_mix": gain(ks[2], (DEPTH, D_MODEL)),
        "w_in": nrm(ks[3], (DEPTH, D_MODEL, IN_COLS), D_MODEL ** -0.5),
        "b_forget": 3.0 + 0.5 * jax.random.normal(ks[4], (DEPTH, FOX_HEADS), f32),
        "lambda_q1": nrm(ks[5], (DEPTH, HEAD_DIM), 0.1),
        "lambda_k1": nrm(ks[6], (DEPTH, HEAD_DIM), 0.1),
        "lambda_q2": nrm(ks[7], (DEPTH, HEAD_DIM), 0.1),
        "lambda_k2": nrm(ks[8], (DEPTH, HEAD_DIM), 0.1),
        "diff_subln": gain(ks[9], (DEPTH, DIFF_V)),
        "ret_gn": gain(ks[10], (DEPTH, WC)),
        "w_out": nrm(ks[11], (DEPTH, MIX_WIDTH, D_MODEL), MIX_WIDTH ** -0.5),
        "norm_ffn": gain(ks[12], (DEPTH, D_MODEL)),
        "dense_w_gate": nrm(ks[13], (N_DENSE, D_MODEL, D_FF_DENSE), D_MODEL ** -0.5),
        "dense_w_up": nrm(ks[14], (N_DENSE, D_MODEL, D_FF_DENSE), D_MODEL ** -0.5),
        "dense_w_down": nrm(ks[15], (N_DENSE, D_FF_DENSE, D_MODEL), D_FF_DENSE ** -0.5),
        "router": nrm(ks[16], (N_MOE, D_MODEL, N_EXPERTS), D_MODEL ** -0.5),
        "moe_w_gate": nrm(ks[17], (N_MOE, N_EXPERTS, D_MODEL, D_FF_EXPERT), D_MODEL ** -0.5),
        "moe_w_up": nrm(ks[18], (N_MOE, N_EXPERTS, D_MODEL, D_FF_EXPERT), D_MODEL ** -0.5),
        "moe_w_down": nrm(ks[19], (N_MOE, N_EXPERTS, D_FF_EXPERT, D_MODEL), D_FF_EXPERT ** -0.5),
        "ple_norm": gain(ks[20], (DEPTH, D_MODEL)),
        "ple_gate": nrm(ks[21], (DEPTH, D_MODEL, D_MODEL), D_MODEL ** -0.5),
        "ple_proj": nrm(ks[22], (DEPTH, PLE_DIM, D_MODEL), PLE_DIM ** -0.5),
        "final_norm": gain(ks[23], (D_MODEL,)),
    }


def reference(x, p, norm_mix, w_in, b_forget, lambda_q1, lambda_k1, lambda_q2, lambda_k2,
              diff_subln, ret_gn, w_out, norm_ffn, dense_w_gate, dense_w_up, dense_w_down,
              router, moe_w_gate, moe_w_up, moe_w_down, ple_norm, ple_gate, ple_proj, final_norm):
    h = x
    for i in range(DEPTH):
        xn = rmsnorm(h, norm_mix[i])
        h = h + hybrid_mixer(xn, w_in[i], b_forget[i], lambda_q1[i], lambda_k1[i], lambda_q2[i],
                             lambda_k2[i], diff_subln[i], ret_gn[i], w_out[i], i)
        xn = rmsnorm(h, norm_ffn[i])
        j = i // 2
        if i % 2 == 0:
            h = h + swiglu(xn, dense_w_gate[j], dense_w_up[j], dense_w_down[j])
        else:
            h = h + moe_swiglu(xn, router[j], moe_w_gate[j], moe_w_up[j], moe_w_down[j])
        gate = jax.nn.sigmoid(rmsnorm(h, ple_norm[i]) @ ple_gate[i])
        h = h + gate * (p[i] @ ple_proj[i])
    return rmsnorm(h, final_norm)
```

```python
import math
import numpy as np
import concourse.bass as bass
import concourse.mybir as mybir
from concourse.bass_utils import run_bass_kernel_spmd

F32 = mybir.dt.float32
BF16 = mybir.dt.bfloat16
AF = mybir.ActivationFunctionType
ALU = mybir.AluOpType
AX = mybir.AxisListType

D = 1024
NT = 2048
NB = 16
IN_COLS = 3332
NEG = -30000.0
EPS = 1e-6
COMPUTE = ("pe", "act", "dve", "pool")
DEBUG = False
FUSED = True
STOP = None
NCORES = 8
SKIP_AB = False
XBAR = False
JOBBAR = False
NJOBS = None
SILU_FN = None


class Res:
    __slots__ = ("name", "ws", "r", "pr")

    def __init__(self, name=""):
        self.name = name
        self.ws = []
        self.r = []
        self.pr = []


class Op:
    __slots__ = ("eng", "fn", "deps", "signal", "sig", "is_dma", "dsem", "dval", "prev", "bar", "inc", "pos")

    def __init__(self, eng, fn, is_dma):
        self.eng = eng
        self.fn = fn
        self.deps = []
        self.signal = False
        self.sig = None
        self.is_dma = is_dma
        self.dsem = None
        self.dval = 0
        self.prev = None
        self.bar = None
        self.inc = 16


class Prog:
    def __init__(self, nc, n_dma_sems=32):
        self.nc = nc
        self.q = {e: [] for e in ("pe", "act", "dve", "pool", "sp")}
        self.esem = {e: nc.alloc_semaphore(f"s_{e}") for e in COMPUTE}
        self.dsems = [nc.alloc_semaphore(f"d_{i}") for i in range(n_dma_sems)]
        self.dcount = [0] * n_dma_sems
        self.dlast = [None] * n_dma_sems
        self.drr = 0
        self.out_dmas = []

    def _add(self, eng, fn, reads, writes, pwrites=(), is_dma=False, inc=16):
        op = Op(eng, fn, is_dma)
        op.inc = inc
        deps = {}
        for r in reads:
            for w in r.ws:
                deps[id(w)] = (w, True)
            r.r.append(op)
        for w in writes:
            for x in w.ws + w.r + w.pr:
                deps.setdefault(id(x), (x, False))
            w.ws = [op]
            w.r = []
            w.pr = []
        for w in pwrites:
            if w.r:
                w.pr = w.r
                w.r = []
                w.ws = []
            for x in w.pr:
                deps.setdefault(id(x), (x, False))
            w.ws.append(op)
        latest = {}
        for d, raw in deps.values():
            if d is op:
                continue
            if (not d.is_dma) and (not is_dma) and d.eng == eng:
                if not raw or eng == "pe":
                    continue
            if d.is_dma:
                key = ("d", d.dsem)
                if key not in latest or d.dval > latest[key].dval:
                    latest[key] = d
            else:
                key = ("e", d.eng)
                if key not in latest or d.pos > latest[key].pos:
                    latest[key] = d
        for d in latest.values():
            d.signal = True
            op.deps.append(d)
        if is_dma:
            s = self.drr
            self.drr = (self.drr + 1) % len(self.dsems)
            op.prev = self.dlast[s]
            self.dcount[s] += inc
            op.dsem = s
            op.dval = self.dcount[s]
            self.dlast[s] = op
        op.pos = len(self.q[eng])
        self.q[eng].append(op)
        return op

    def pe(self, fn, reads=(), writes=(), pwrites=()):
        return self._add("pe", fn, reads, writes, pwrites)

    def act(self, fn, reads=(), writes=(), pwrites=()):
        return self._add("act", fn, reads, writes, pwrites)

    def dve(self, fn, reads=(), writes=(), pwrites=()):
        return self._add("dve", fn, reads, writes, pwrites)

    def pool(self, fn, reads=(), writes=(), pwrites=()):
        return self._add("pool", fn, reads, writes, pwrites)

    def dma(self, out, in_, reads=(), writes=(), pwrites=(), q="sp", is_out=False):
        op = self._add(q, lambda e: e.dma_start(out=out, in_=in_), reads, writes, pwrites, is_dma=True)
        if is_out:
            self.out_dmas.append(op)
        return op

    def coll(self, out, in_, reads=(), writes=()):
        groups = [[0, 1], [2, 3], [4, 5], [6, 7]]
        return self._add("pool", lambda e: e.collective_compute("AllGather", op=ALU.bypass, replica_groups=groups,
                                                                 ins=[in_.opt()], outs=[out.opt()]),
                         reads, writes, is_dma=True, inc=1)

    def barrier(self):
        lasts = []
        for e in COMPUTE:
            for op in reversed(self.q[e]):
                if not op.is_dma and op.bar is None:
                    op.signal = True
                    lasts.append(op)
                    break
        dvals = list(self.dcount)
        for e in ("pe", "act", "dve", "pool", "sp"):
            m = Op(e, None, False)
            m.bar = (lasts, dvals)
            self.q[e].append(m)

    def emit(self):
        nc = self.nc
        for e in COMPUTE:
            k = 0
            for op in self.q[e]:
                if op.bar is None and not op.is_dma and op.signal:
                    k += 1
                    op.sig = k
        finals = list(self.out_dmas)
        prog = self

        def run(engname, eng):
            waited = {}

            def wait(key, val):
                if waited.get(key, 0) >= val:
                    return
                waited[key] = val
                sem = prog.dsems[key[1]] if key[0] == "d" else prog.esem[key[1]]
                eng.wait_ge(sem, val)

            for op in prog.q[engname]:
                if op.bar is not None:
                    lasts, dvals = op.bar
                    for d in lasts:
                        wait(("e", d.eng), d.sig)
                    for s, v in enumerate(dvals):
                        if v:
                            wait(("d", s), v)
                    continue
                waits = {}
                for d in op.deps:
                    if d.is_dma:
                        key = ("d", d.dsem)
                        val = d.dval
                    else:
                        key = ("e", d.eng)
                        val = d.sig
                    if val > waits.get(key, 0):
                        waits[key] = val
                if op.is_dma and op.prev is not None:
                    key = ("d", op.dsem)
                    if op.prev.dval > waits.get(key, 0):
                        waits[key] = op.prev.dval
                for key, val in waits.items():
                    wait(key, val)
                inst = op.fn(eng)
                if op.is_dma:
                    if op.inc == 16:
                        inst.then_inc(prog.dsems[op.dsem], 16)
                    else:
                        inst.then_inc(prog.dsems[op.dsem])
                elif op.signal:
                    inst.then_inc(prog.esem[op.eng], 1)
            if engname == "sp":
                for d in finals:
                    wait(("d", d.dsem), d.dval)

        with nc.Block() as block:
            @block.tensor
            def _(e):
                run("pe", e)

            @block.scalar
            def _(e):
                run("act", e)

            @block.vector
            def _(e):
                run("dve", e)

            @block.gpsimd
            def _(e):
                run("pool", e)

            @block.sync
            def _(e):
                run("sp", e)


class Arena:
    def __init__(self, nc, words):
        self.t = nc.alloc_sbuf_tensor("arena", [128, words], F32)
        self.words = words
        self.off = 0
        self.base = 0

    def f32(self, n):
        a = self.off
        self.off += (n + 15) // 16 * 16
        assert self.off <= self.words, f"arena overflow {self.off} > {self.words}"
        return self.t[:, a:a + n]

    def bf16(self, n):
        w = (n + 1) // 2
        a = self.off
        self.off += (w + 15) // 16 * 16
        assert self.off <= self.words, f"arena overflow {self.off} > {self.words}"
        return self.t[:, a:a + w].bitcast(BF16)[:, 0:n]

    def mark_persistent(self):
        self.base = self.off

    def reset(self):
        self.off = self.base


CST_LAYOUT = {}


def _cst_layout():
    off = 0
    for name, n in [("ident", 128), ("maskE", 128), ("maskO", 128), ("innerT", 512), ("tri", 128),
                    ("pfx", 128), ("e127", 1), ("gsel", 2), ("kdec", 4), ("qdecT", 512),
                    ("cdecT", 256), ("ones", 128)]:
        CST_LAYOUT[name] = (off, n)
        off += n
    return off


CST_W = _cst_layout()


def make_consts(g):
    c = np.zeros((128, CST_W), np.float32)

    def put(name, arr):
        o, n = CST_LAYOUT[name]
        arr = np.asarray(arr, np.float32)
        c[:arr.shape[0], o:o + n] = arr.reshape(arr.shape[0], n)

    put("ident", np.eye(128))
    k = np.arange(128)[:, None]
    q = np.arange(128)[None, :]
    causal = np.where(k <= q, 0.0, NEG)
    if g == 0:
        put("maskE", causal)
        put("maskO", np.full((128, 128), NEG))
    else:
        put("maskE", np.zeros((128, 128)))
        put("maskO", causal)
    hh = np.arange(4, dtype=np.float64)
    log_g = np.log1p(-np.exp2(-5.0 - hh))
    diff = (q - k).astype(np.float64)
    innerT = np.where(diff[None] >= 0, np.exp(diff[None] * log_g[:, None, None]), 0.0)
    put("innerT", innerT.transpose(1, 0, 2).reshape(128, 512))
    put("tri", (k <= q).astype(np.float32))
    idx = np.arange(128)
    blk, hd = idx // 4, idx % 4
    pfx = ((hd[:, None] == hd[None, :]) & (blk[:, None] < blk[None, :])).astype(np.float32)
    put("pfx", pfx)
    e127 = np.zeros((128, 1)); e127[127, 0] = 1.0
    put("e127", e127)
    put("gsel", np.tile(np.array([[1.0 - g, float(g)]]), (128, 1)))
    j = np.arange(128, dtype=np.float64)
    kdec = np.exp((127.0 - j)[:, None] * log_g[None, :])
    put("kdec", kdec)
    qdec = np.exp((j + 1.0)[None, :] * log_g[:, None])
    put("qdecT", np.tile(qdec.reshape(1, 512), (64, 1)))
    cdec = np.exp(128.0 * log_g)
    put("cdecT", np.tile(np.repeat(cdec, 64)[None, :], (64, 1)))
    put("ones", np.ones((128, 128)))
    half = 32
    inv = (10000.0 ** (-np.arange(half, dtype=np.float32) / half)).astype(np.float32)
    i = np.arange(NB)[:, None]
    jj = np.arange(128)[None, :]
    pos = ((2 * i + g) * 128 + jj).reshape(-1).astype(np.float32)
    ang = pos[:, None] * inv[None, :]
    cs = np.concatenate([np.cos(ang), np.sin(ang)], axis=1).astype(np.float32)
    return c, cs


WEIGHT_SPECS = {
    "norm_mix": [2, 1024], "w_in": [2, 1024, IN_COLS], "b_forget": [2, 4],
    "lambda_q1": [2, 64], "lambda_k1": [2, 64], "lambda_q2": [2, 64], "lambda_k2": [2, 64],
    "diff_subln": [2, 128], "ret_gn": [2, 256], "w_out": [2, 1024, 1024], "norm_ffn": [2, 1024],
    "dense_w_gate": [1, 1024, 2816], "dense_w_up": [1, 1024, 2816], "dense_w_down": [1, 2816, 1024],
    "router": [1, 1024, 8], "moe_w_gate": [1, 8, 1024, 3584], "moe_w_up": [1, 8, 1024, 3584],
    "moe_w_down": [1, 8, 3584, 1024], "ple_norm": [2, 1024], "ple_gate": [2, 1024, 1024],
    "ple_proj": [2, 256, 1024], "final_norm": [1024],
}

SCRATCH = {
    "QTA": ([8, 64, NT], BF16), "QTB": ([4, 65, NT], BF16), "CB": ([NT, 1024], BF16),
    "KTA_own": ([8, 64, NT], BF16), "KTB_own": ([4, 65, NT], BF16),
    "VA_own": ([NT, 512], BF16), "VB_own": ([NT, 4, 65], BF16),
    "LF_own": ([128, 16, 4], F32), "KV_own": ([16, 64, 256], F32),
    "KTA_all": ([2, 8, 64, NT], BF16), "KTB_all": ([2, 4, 65, NT], BF16),
    "VA_all": ([2, NT, 512], BF16), "VB_all": ([2, NT, 4, 65], BF16),
    "LF_all": ([2, 128, 16, 4], F32), "KV_all": ([2, 16, 64, 256], F32),
    "mixT": ([1024, NT], BF16), "hb": ([NT, 1024], F32), "QTBc": ([4, NT], BF16),
    "routerT": ([8, 1024], F32),
}
OWN = ["KTA", "KTB", "VA", "VB", "LF", "KV"]
LOCAL = ["QTA", "QTB", "CB"]


class LazyW(dict):
    def __init__(self, nc):
        super().__init__()
        self.nc = nc
        self.specs = dict(WEIGHT_SPECS)
        self.specs.update({"p": [2, NT, 256], "cst": [128, CST_W], "cs": [NT, 64]})

    def __missing__(self, n):
        ap = self.nc.dram_tensor(n, self.specs[n], F32, kind="ExternalInput").ap()
        self[n] = ap
        return ap


class MK:
    def __init__(self, stage):
        self.stage = stage
        nc = self.nc = bass.Bass("TRN2", target_bir_lowering=False)
        self.P = Prog(nc)
        self.dr = {}
        self.rd = {}
        ext_in, ext_out = [], []
        if stage == 0:
            ext_in = ["x"]
            ext_out = [n + "_own" for n in OWN] + LOCAL
        elif stage == 1:
            ext_in = ["x"] + [n + "_all" for n in OWN] + LOCAL
            ext_out = [n + "_own" for n in OWN] + ["QTA2", "QTB2", "CB2", "hb"] + (["mixT", "dbg1", "dbg2"] if DEBUG else [])
        elif stage == 2:
            ext_in = ["hb"] + [n + "_all" for n in OWN] + LOCAL
            ext_out = ["out"]
        else:
            ext_in = ["x"]
            ext_out = ["out"]
        self.ext_in, self.ext_out = ext_in, ext_out
        self.w = LazyW(nc)

        def mk(name, shape, dt):
            if name in ext_in:
                kind = "ExternalInput"
            elif name in ext_out:
                kind = "ExternalOutput"
            else:
                kind = "Internal"
            self.dr[name] = nc.dram_tensor(name, shape, dt, kind=kind).ap()
            self.rd[name] = Res(name)

        if "x" in ext_in:
            mk("x", [NT, 1024], F32)
        if "dbg1" in ext_out:
            mk("dbg1", [NT, 1024], F32)
            mk("dbg2", [NT, 1024], F32)
        if "out" in ext_out:
            mk("out", [NT, 1024], F32)
        for n, (shp, dt) in SCRATCH.items():
            if stage == 1 and n in LOCAL:
                mk(n, shp, dt)
                mk(n + "2", shp, dt)
            elif stage == 0 and (n.endswith("_all") or n in ("mixT", "hb", "QTBc", "routerT")):
                continue
            elif stage == 2 and (n.endswith("_own")):
                continue
            else:
                mk(n, shp, dt)
        self.A = Arena(nc, 52000)
        self.psum = nc.alloc_psum_tensor("psum", [128, 8, 512], F32)
        self.rps = [Res(f"ps{i}") for i in range(8)]
        self.setup_persistent()

    def ps(self, b):
        return self.psum[:, b, :]

    def psb(self, b):
        return self.psum[:, b, :].bitcast(BF16)

    def cst(self, name, rows=128):
        o, n = CST_LAYOUT[name]
        return self.cf[0:rows, o:o + n]

    def bcast_load(self, dst, src1d, res):
        b = src1d.partition_broadcast(128)
        if len(b.shape) == 3:
            b = b.rearrange("p a n -> p (a n)")
        self.P.dma(dst, b, writes=[res])

    def setup_persistent(self):
        A, P = self.A, self.P
        self.hs = A.f32(16 * 1024).rearrange("p (i n) -> p i n", i=16)
        self.rhs = [Res(f"hs{i}") for i in range(16)]
        self.cf = A.f32(CST_W)
        self.rcf = Res("cf")
        P.dma(self.cf, self.w["cst"], writes=[self.rcf])
        self.identb = A.bf16(128)
        self.maskEb = A.bf16(128)
        self.maskOb = A.bf16(128)
        self.onesb = A.bf16(128)
        self.rcb = Res("cb")
        P.dve(lambda e: e.tensor_copy(self.identb, self.cst("ident")), [self.rcf], pwrites=[self.rcb])
        P.dve(lambda e: e.tensor_copy(self.maskEb, self.cst("maskE")), [self.rcf], pwrites=[self.rcb])
        P.dve(lambda e: e.tensor_copy(self.maskOb, self.cst("maskO")), [self.rcf], pwrites=[self.rcb])
        P.dve(lambda e: e.tensor_copy(self.onesb, self.cst("ones")), [self.rcf], pwrites=[self.rcb])
        A.mark_persistent()

    def load_h(self, src_name):
        src = self.dr[src_name]
        for i in range(16):
            self.P.dma(self.hs[:, i, :], src[i * 128:(i + 1) * 128, :], reads=[self.rd[src_name]], writes=[self.rhs[i]])

    def store_h(self, dst_name, is_out=False):
        dst = self.dr[dst_name]
        for i in range(16):
            self.P.dma(dst[i * 128:(i + 1) * 128, :], self.hs[:, i, :], reads=[self.rhs[i]],
                       pwrites=[self.rd[dst_name]], is_out=is_out)

    def rms_stats(self, rstd, r_rstd, scratch, r_scratch):
        P = self.P
        P.dve(lambda e: e.memset(rstd, 0.0), [], [r_rstd])
        for i in range(16):
            P.act(lambda e, i=i: e.activation(scratch, self.hs[:, i, :], AF.Square, scale=1.0 / 32,
                                              accum_out=rstd[:, i:i + 1]),
                  [self.rhs[i]] + ([r_rstd] if i == 0 else []), [r_scratch], pwrites=[r_rstd])
        P.dve(lambda e: e.tensor_scalar(rstd, rstd, EPS, None, ALU.add), [r_rstd], [r_rstd])
        P.act(lambda e: e.activation(rstd, rstd, AF.Ln), [r_rstd], [r_rstd])
        P.act(lambda e: e.activation(rstd, rstd, AF.Exp, scale=-0.5), [r_rstd], [r_rstd])

    def norm_transpose(self, i, rstd, r_rstd, gt, r_gt, xnb, r_xnb, xnT, r_xnT, bank, xnf=None, r_xnf=None):
        P = self.P
        if xnf is not None:
            P.dve(lambda e: e.scalar_tensor_tensor(xnf, self.hs[:, i, :], rstd[:, i:i + 1], gt, ALU.mult, ALU.mult),
                  [self.rhs[i], r_rstd, r_gt], [r_xnf])
            P.act(lambda e: e.copy(xnb, xnf), [r_xnf], [r_xnb])
        else:
            P.dve(lambda e: e.scalar_tensor_tensor(xnb, self.hs[:, i, :], rstd[:, i:i + 1], gt, ALU.mult, ALU.mult),
                  [self.rhs[i], r_rstd, r_gt], [r_xnb])
        pb = self.psb(bank).rearrange("p (k n) -> p k n", k=8)
        for k in range(8):
            P.pe(lambda e, k=k: e.transpose(pb[:, k, :], xnb[:, k * 128:(k + 1) * 128], self.identb),
                 [r_xnb, self.rcb], pwrites=[self.rps[bank]])
        P.act(lambda e: e.copy(xnT[:, :, i * 128:(i + 1) * 128], pb), [self.rps[bank]], pwrites=[r_xnT])

    def wload(self, dst, src, res):
        self.P.dma(dst, src, writes=[res], q="pool")

    def phase1(self, L, sfx=""):
        A, P, w, dr, rd = self.A, self.P, self.w, self.dr, self.rd
        A.reset()
        QTA, QTB, CB = dr["QTA" + sfx], dr["QTB" + sfx], dr["CB" + sfx]
        rQTA, rQTB, rCB = rd["QTA" + sfx], rd["QTB" + sfx], rd["CB" + sfx]
        gt = A.f32(1024); r_gt = Res()
        self.bcast_load(gt, w["norm_mix"][L, :], r_gt)
        rstd = A.f32(16); r_rstd = Res()
        scr = A.f32(1024); r_scr = Res()
        self.rms_stats(rstd, r_rstd, scr, r_scr)
        xnT = A.bf16(8 * NT).rearrange("p (k t) -> p k t", k=8); r_xnT = Res()
        xnb = [A.bf16(1024) for _ in range(2)]; r_xnb = [Res(), Res()]
        win = w["w_in"][L]
        fm_groups = [(512, "kA"), (1536, "qkB"), (0, "qA")]
        wfm = [A.bf16(8 * 512).rearrange("p (k n) -> p k n", k=8) for _ in range(2)]; r_wfm = [Res(), Res()]
        wtm = {}
        r_wtm = {}
        for name, c0, n in [("vA", 1024, 512), ("vB", 2048, 260), ("qkC", 2308, 512), ("vgC", 2820, 512)]:
            wtm[name] = A.bf16(8 * n).rearrange("p (k n) -> p k n", k=8)
            r_wtm[name] = Res()
        for name, c0, n in [("vA", 1024, 512), ("vB", 2048, 260), ("qkC", 2308, 512), ("vgC", 2820, 512)]:
            self.wload(wtm[name], win[:, c0:c0 + n].rearrange("(k p) n -> p k n", p=128), r_wtm[name])
        for i in range(16):
            self.norm_transpose(i, rstd, r_rstd, gt, r_gt, xnb[i % 2], r_xnb[i % 2], xnT, r_xnT, 7)
        for gi in range(2):
            c0 = fm_groups[gi][0]
            self.wload(wfm[gi], win[:, c0:c0 + 512].rearrange("(k p) n -> p k n", p=128), r_wfm[gi])
        if XBAR:
            P.barrier()
        cs = A.f32(16 * 64).rearrange("p (i n) -> p i n", i=16); r_cs = Res()
        P.dma(cs, w["cs"].rearrange("(i p) n -> p i n", p=128), writes=[r_cs])
        bft = A.f32(4); r_bft = Res()
        self.bcast_load(bft, w["b_forget"][L, :], r_bft)
        xfb = A.f32(64); r_xfb = Res()
        vast = [A.bf16(512) for _ in range(2)]; r_vast = [Res(), Res()]
        vbst = [A.bf16(4 * 65).rearrange("p (h n) -> p h n", h=4) for _ in range(2)]; r_vbst = [Res(), Res()]
        for b in range(2):
            P.dve(lambda e, b=b: e.memset(vbst[b], 1.0), [], [r_vbst[b]])
        cbt = [A.bf16(1024) for _ in range(2)]; r_cbt = [Res(), Res()]
        tmp = [A.f32(256) for _ in range(4)]; r_tmp = [Res() for _ in range(4)]
        kd = [A.bf16(256) for _ in range(2)]; r_kd = [Res(), Res()]
        kvst = [A.f32(256) for _ in range(2)]; r_kvst = [Res(), Res()]
        kdec = self.cst("kdec")
        kv_pending = []
        for i in range(16):
            b = i % 2
            bqk, bvg = 2 + b, 4 + b
            tl = slice(i * 128, (i + 1) * 128)
            for k in range(8):
                for (name, bank, n) in (("vA", 0, 512), ("vB", 1, 260), ("qkC", bqk, 512), ("vgC", bvg, 512)):
                    P.pe(lambda e, k=k, name=name, bank=bank, n=n, tl=tl: e.matmul(
                        self.ps(bank)[:, 0:n], xnT[:, k, tl], wtm[name][:, k, :], start=(k == 0), stop=(k == 7)),
                        [r_wtm[name], r_xnT], writes=[self.rps[bank]] if k == 0 else (),
                        pwrites=() if k == 0 else [self.rps[bank]])
            while kv_pending:
                kv_pending.pop(0)()
            P.act(lambda e, b=b: e.copy(vast[b], self.ps(0)), [self.rps[0]], [r_vast[b]])
            P.dma(dr["VA_own"][tl, :], vast[b], reads=[r_vast[b]], pwrites=[rd["VA_own"]])
            P.act(lambda e, b=b: e.copy(vbst[b][:, :, 1:65], self.ps(1)[:, 0:256].rearrange("p (h n) -> p h n", h=4)),
                  [self.rps[1], r_vbst[b]], pwrites=[r_vbst[b]])
            P.dve(lambda e, i=i: e.tensor_tensor(xfb[:, i * 4:(i + 1) * 4], self.ps(1)[:, 256:260], bft, ALU.add),
                  [self.rps[1], r_bft], pwrites=[r_xfb])
            P.dma(dr["VB_own"][tl, :, :], vbst[b], reads=[r_vbst[b]], pwrites=[rd["VB_own"]])
            pq = self.ps(bqk).rearrange("p (s two n) -> p s two n", s=8, two=2)
            x1, x2 = pq[:, :, 0, :], pq[:, :, 1, :]
            cosb = cs[:, i, 0:32].unsqueeze(1).broadcast_to([128, 8, 32])
            sinb = cs[:, i, 32:64].unsqueeze(1).broadcast_to([128, 8, 32])
            t = [tt.rearrange("p (s n) -> p s n", s=8) for tt in tmp]
            rot = cbt[b][:, 0:512].rearrange("p (s two n) -> p s two n", s=8, two=2)
            P.dve(lambda e, x1=x1, cosb=cosb: e.tensor_tensor(t[0], x1, cosb, ALU.mult), [self.rps[bqk], r_cs], [r_tmp[0]])
            P.dve(lambda e, x2=x2, sinb=sinb: e.tensor_tensor(t[1], x2, sinb, ALU.mult), [self.rps[bqk], r_cs], [r_tmp[1]])
            P.dve(lambda e, x1=x1, sinb=sinb: e.tensor_tensor(t[2], x1, sinb, ALU.mult), [self.rps[bqk], r_cs], [r_tmp[2]])
            P.dve(lambda e, x2=x2, cosb=cosb: e.tensor_tensor(t[3], x2, cosb, ALU.mult), [self.rps[bqk], r_cs], [r_tmp[3]])
            P.pool(lambda e, rot=rot: e.tensor_tensor(rot[:, :, 0, :], t[0], t[1], ALU.subtract),
                   [r_tmp[0], r_tmp[1]], pwrites=[r_cbt[b]])
            P.pool(lambda e, rot=rot: e.tensor_tensor(rot[:, :, 1, :], t[2], t[3], ALU.add),
                   [r_tmp[2], r_tmp[3]], pwrites=[r_cbt[b]])
            P.act(lambda e, b=b, bvg=bvg: e.copy(cbt[b][:, 512:1024], self.ps(bvg)), [self.rps[bvg]], pwrites=[r_cbt[b]])
            rk = cbt[b][:, 256:512].rearrange("p (h n) -> p h n", h=4)
            P.dve(lambda e, b=b, rk=rk: e.tensor_tensor(kd[b].rearrange("p (h n) -> p h n", h=4), rk,
                                                         kdec.unsqueeze(2).broadcast_to([128, 4, 64]), ALU.mult),
                  [r_cbt[b], self.rcf], [r_kd[b]])
            def kvjob(i=i, b=b):
                for hh in range(4):
                    P.pe(lambda e, hh=hh, b=b: e.matmul(self.ps(6)[0:64, hh * 64:(hh + 1) * 64], kd[b][:, hh * 64:(hh + 1) * 64],
                                                        cbt[b][:, 512 + hh * 64:512 + (hh + 1) * 64], start=True, stop=True),
                         [r_kd[b], r_cbt[b]], writes=[self.rps[6]] if hh == 0 else (), pwrites=() if hh == 0 else [self.rps[6]])
                P.act(lambda e, b=b: e.copy(kvst[b][0:64, :], self.ps(6)[0:64, 0:256]), [self.rps[6]], [r_kvst[b]])
                P.dma(dr["KV_own"][i], kvst[b][0:64, :], reads=[r_kvst[b]], pwrites=[rd["KV_own"]])
            kv_pending.append(kvjob)
            P.dma(CB[tl, :], cbt[b], reads=[r_cbt[b]], pwrites=[rCB])
        while kv_pending:
            kv_pending.pop(0)()
        lf = A.f32(64); r_lf = Res()
        P.act(lambda e: e.activation(lf, xfb, AF.Exp, scale=-1.0), [r_xfb], [r_lf])
        P.dve(lambda e: e.tensor_scalar(lf, lf, 1.0, None, ALU.add), [r_lf], [r_lf])
        P.act(lambda e: e.activation(lf, lf, AF.Ln), [r_lf], [r_lf])
        P.dve(lambda e: e.tensor_scalar(lf, lf, -1.0, None, ALU.mult), [r_lf], [r_lf])
        P.dma(dr["LF_own"], lf.rearrange("p (i h) -> p i h", i=16), reads=[r_lf], writes=[rd["LF_own"]])
        if self.stage == "all":
            for n_ in ("VA", "VB", "LF", "KV"):
                self.coll_one(n_)
        stage = [A.bf16(NT) for _ in range(2)]; r_stage = [Res(), Res()]
        onesrow = A.bf16(NT); r_ones = Res()
        P.dve(lambda e: e.memset(onesrow[0:4, :], 1.0), [], [r_ones])
        P.dma(dr["KTB_own"][:, 64, :], onesrow[0:4, :], reads=[r_ones], pwrites=[rd["KTB_own"]])
        cnt = 0
        for gi, (c0, kind) in enumerate(fm_groups):
            wt, r_wt = wfm[gi % 2], r_wfm[gi % 2]
            for c in range(4):
                st, r_st = stage[cnt % 2], r_stage[cnt % 2]
                isq = (kind == "qA") or (kind == "qkB" and c < 2)
                for T in range(4):
                    bank = (cnt * 4 + T) % 6
                    for k in range(8):
                        P.pe(lambda e, k=k, c=c, T=T, bank=bank, wt=wt: e.matmul(
                            self.ps(bank), wt[:, k, c * 128:(c + 1) * 128], xnT[:, k, T * 512:(T + 1) * 512],
                            start=(k == 0), stop=(k == 7)),
                            [r_wt, r_xnT], writes=[self.rps[bank]] if k == 0 else (), pwrites=() if k == 0 else [self.rps[bank]])
                    sc = 0.125 if isq else 1.0
                    if T % 2 == 0:
                        P.act(lambda e, bank=bank, st=st, T=T, sc=sc: e.activation(
                            st[:, T * 512:(T + 1) * 512], self.ps(bank), AF.Copy, scale=sc),
                            [self.rps[bank]], pwrites=[r_st])
                    else:
                        P.dve(lambda e, bank=bank, st=st, T=T, sc=sc: e.tensor_scalar(
                            st[:, T * 512:(T + 1) * 512], self.ps(bank), sc, None, ALU.mult),
                            [self.rps[bank]], pwrites=[r_st])
                if kind == "qA":
                    P.dma(QTA[2 * c:2 * c + 2].rearrange("m d t -> (m d) t"), st, reads=[r_st], pwrites=[rQTA])
                elif kind == "kA":
                    P.dma(dr["KTA_own"][2 * c:2 * c + 2].rearrange("m d t -> (m d) t"), st, reads=[r_st],
                          pwrites=[rd["KTA_own"]])
                else:
                    dst, rdst = (QTB, rQTB) if c < 2 else (dr["KTB_own"], rd["KTB_own"])
                    cc = c % 2
                    P.dma(dst[2 * cc, 0:64, :], st[0:64, :], reads=[r_st], pwrites=[rdst])
                    P.dma(dst[2 * cc + 1, 0:64, :], st[64:128, :], reads=[r_st], pwrites=[rdst])
                cnt += 1
            if self.stage == "all" and kind == "kA":
                self.coll_one("KTA")
            if self.stage == "all" and kind == "qkB":
                self.coll_one("KTB")
            if gi + 2 < len(fm_groups):
                c0n = fm_groups[gi + 2][0]
                self.wload(wfm[gi % 2], win[:, c0n:c0n + 512].rearrange("(k p) n -> p k n", p=128), r_wfm[gi % 2])
        P.barrier()

    def phase2(self, L):
        A, P, w, dr, rd = self.A, self.P, self.w, self.dr, self.rd
        A.reset()
        lam_init = 0.8 - 0.6 * math.exp(-0.3 * L)
        lq = A.f32(256); r_lq = Res()
        for n, name in enumerate(["lambda_q1", "lambda_k1", "lambda_q2", "lambda_k2"]):
            b = w[name][L, :].partition_broadcast(128)
            if len(b.shape) == 3:
                b = b.rearrange("p a n -> p (a n)")
            P.dma(lq[:, n * 64:(n + 1) * 64], b, pwrites=[r_lq])
        lt = A.f32(128); r_lt = Res()
        lam = A.f32(4); r_lam = Res()
        P.dve(lambda e: e.memset(lam, 0.0), [], [r_lam])
        P.dve(lambda e: e.scalar_tensor_tensor(lt[:, 0:64], lq[:, 0:64], 1.0, lq[:, 64:128], ALU.mult, ALU.mult,
                                               accum_out=lam[:, 0:1]), [r_lq, r_lam], [r_lt], pwrites=[r_lam])
        P.dve(lambda e: e.scalar_tensor_tensor(lt[:, 64:128], lq[:, 128:192], 1.0, lq[:, 192:256], ALU.mult, ALU.mult,
                                               accum_out=lam[:, 1:2]), [r_lq], pwrites=[r_lt, r_lam])
        P.act(lambda e: e.activation(lam[:, 0:2], lam[:, 0:2], AF.Exp), [r_lam], [r_lam])
        P.dve(lambda e: e.tensor_tensor(lam[:, 2:3], lam[:, 1:2], lam[:, 0:1], ALU.subtract), [r_lam], [r_lam])
        P.dve(lambda e: e.tensor_scalar(lam[:, 2:3], lam[:, 2:3], -lam_init, None, ALU.add), [r_lam], [r_lam])
        neglam = lam[:, 2:3]
        gcol = A.f32(1); r_gcol = Res()
        P.dma(gcol, w["diff_subln"][L, :].rearrange("(p o) -> p o", o=1), writes=[r_gcol])
        P.dve(lambda e: e.tensor_scalar(gcol, gcol, 1.0 - lam_init, None, ALU.mult), [r_gcol], [r_gcol])

        lfS = A.f32(128); r_lfS = Res()
        lfv = lfS.rearrange("p (i r h) -> p i r h", i=16, r=2)
        for r in range(2):
            P.dma(lfv[:, :, r, :], dr["LF_all"][r], reads=[rd["LF_all"]], pwrites=[r_lfS])
        c1 = A.f32(128); r_c1 = Res()
        P.pe(lambda e: e.matmul(self.ps(7)[:, 0:128], self.cst("tri"), lfS, start=True, stop=True),
             [self.rcf, r_lfS], [self.rps[7]])
        P.dve(lambda e: e.tensor_copy(c1, self.ps(7)[:, 0:128]), [self.rps[7]], [r_c1])
        P.pe(lambda e: e.matmul(self.ps(7)[:, 128:129], c1, self.cst("e127"), start=True, stop=True),
             [self.rcf, r_c1], [self.rps[7]])
        totc = A.f32(1); r_totc = Res()
        P.dve(lambda e: e.tensor_copy(totc, self.ps(7)[:, 128:129]), [self.rps[7]], [r_totc])
        totB = A.f32(128); r_totB = Res()
        P.dve(lambda e: e.tensor_scalar(totB, self.cst("ones"), totc, None, ALU.mult), [self.rcf, r_totc], [r_totB])
        P.pe(lambda e: e.matmul(self.ps(7)[:, 256:384], totB, self.cst("pfx"), start=True, stop=True),
             [self.rcf, r_totB], [self.rps[7]])
        cum = A.f32(128); r_cum = Res()
        P.dve(lambda e: e.tensor_tensor(cum, c1, self.ps(7)[:, 256:384], ALU.add), [r_c1, self.rps[7]], [r_cum])
        negcum = A.f32(128); r_negcum = Res()
        P.dve(lambda e: e.tensor_scalar(negcum, cum, -1.0, None, ALU.mult), [r_cum], [r_negcum])
        cumv = cum.rearrange("p (i r h) -> p i r h", i=16, r=2)
        cown = A.f32(64); r_cown = Res()
        cownv = cown.rearrange("p (h i) -> p i h", h=4)
        gsel = self.cst("gsel")
        P.dve(lambda e: e.tensor_scalar(cownv, cumv[:, :, 0, :], gsel[:, 0:1], None, ALU.mult), [r_cum, self.rcf], [r_cown])
        P.dve(lambda e: e.scalar_tensor_tensor(cownv, cumv[:, :, 1, :], gsel[:, 1:2], cownv, ALU.mult, ALU.add),
              [r_cum, self.rcf, r_cown], [r_cown])
        P.pe(lambda e: e.transpose(self.ps(7)[0:64, 384:512], cown, self.cst("ident")), [r_cown, self.rcf], [self.rps[7]])
        cT = A.bf16(128); r_cT = Res()
        P.dve(lambda e: e.tensor_copy(cT[0:64, :], self.ps(7)[0:64, 384:512]), [self.rps[7]], [r_cT])
        P.dma(dr["QTBc"].rearrange("h (i t) -> (h i) t", t=128), cT[0:64, :], reads=[r_cT], writes=[rd["QTBc"]])

        if STOP == 'cum':
            P.barrier()
            return
        NBUF = 4
        kT = [A.bf16(2 * NT).rearrange("p (r t) -> p r t", r=2) for _ in range(NBUF)]; r_kT = [Res() for _ in range(NBUF)]
        qT = [A.bf16(NT) for _ in range(NBUF)]; r_qT = [Res() for _ in range(NBUF)]
        vS = [A.bf16(2 * 16 * 128).rearrange("p (r j d) -> p r j d", r=2, j=16) for _ in range(2)]; r_vS = [Res(), Res()]
        for kb in range(NBUF):
            P.pool(lambda e, kb=kb: e.memset(kT[kb][64:128], 0.0), [], [r_kT[kb]])
            P.pool(lambda e, kb=kb: e.memset(qT[kb][64:128], 0.0), [], [r_qT[kb]])
        pT = [A.bf16(512) for _ in range(3)]; r_pT = [Res() for _ in range(3)]
        om = [A.f32(512) for _ in range(2)]; r_om = [Res(), Res()]
        dS = A.f32(512); r_dS = Res()
        rb = A.f32(512); r_rb = Res()
        oa = A.f32(512); r_oa = Res()
        sqb = A.bf16(512); r_sqb = Res()
        rsd = A.f32(512); r_rsd = Res()
        ost = [A.bf16(512) for _ in range(2)]; r_ost = [Res(), Res()]
        onesf = self.cst("ones")
        mixT, rmix = dr["mixT"], rd["mixT"]

        pending = []

        def tick():
            for it in list(pending):
                it[0] -= 1
                if it[0] <= 0:
                    pending.remove(it)
                    it[1]()

        def flush():
            while pending:
                pending.sort(key=lambda it: it[0])
                it = pending.pop(0)
                it[1]()

        state = {"blk": 0, "mapi": 0}

        def run_map(kd, kt, qt, vs, r_kt, r_qt, r_vs, T, accb, denb, isB, hB, post):
            blocks = []
            for r in range(2):
                for j in range(4 * T + 4):
                    c0 = 0 if j < 4 * T else 128 * (j - 4 * T)
                    blocks.append((r, j, c0, j >= 4 * T))
            blocks.sort(key=lambda b: (b[1], b[0]))
            nblk = len(blocks)
            M = 65 if isB else 128

            def qk(n):
                r, j, c0, masked = blocks[n]
                sb = state["blk"] % 3
                state["blk"] += 1
                P.pe(lambda e: e.matmul(self.ps(sb)[:, c0:512], kt[0:kd, r, j * 128:(j + 1) * 128],
                                        qt[0:kd, T * 512 + c0:(T + 1) * 512], start=True, stop=not masked),
                     [r_kt, r_qt], [self.rps[sb]])
                if masked:
                    mk = self.maskEb if r == 0 else self.maskOb
                    P.pe(lambda e: e.matmul(self.ps(sb)[:, c0:c0 + 128], self.identb, mk, start=False, stop=True),
                         [self.rcb], pwrites=[self.rps[sb]])
                pb = sb
                if isB:
                    col = (2 * j + r) * 4 + hB
                    P.act(lambda e: e.activation(pT[pb][:, c0:512], self.ps(sb)[:, c0:512], AF.Exp,
                                                 bias=negcum[:, col:col + 1]),
                          [self.rps[sb], r_negcum], [r_pT[pb]])
                else:
                    P.act(lambda e: e.activation(pT[pb][:, c0:512], self.ps(sb)[:, c0:512], AF.Exp),
                          [self.rps[sb]], [r_pT[pb]])
                return pb

            def pv(n, pb):
                r, j, c0, masked = blocks[n]
                first, last = (n == 0), (n == nblk - 1)
                P.pe(lambda e: e.matmul(self.ps(accb)[0:M, c0:512], vs[:, r, j, 0:M], pT[pb][:, c0:512],
                                        start=first, stop=last),
                     [r_vs, r_pT[pb]], writes=[self.rps[accb]] if first else (), pwrites=() if first else [self.rps[accb]])
                if not isB:
                    P.pe(lambda e: e.matmul(self.ps(denb)[:, c0:512], self.onesb, pT[pb][:, c0:512],
                                            start=first, stop=last),
                         [self.rcb, r_pT[pb]], writes=[self.rps[denb]] if first else (),
                         pwrites=() if first else [self.rps[denb]])

            pbs = [qk(0)]
            if nblk > 1:
                pbs.append(qk(1))
            for n in range(nblk):
                if n + 2 < nblk:
                    pbs.append(qk(n + 2))
                pv(n, pbs[n])
                tick()
            post()

        for h in ([] if SKIP_AB else range(4)):
            vb = h % 2
            P.dma(vS[vb], dr["VA_all"][:, :, h * 128:(h + 1) * 128].rearrange("r (j p) d -> p r j d", p=128),
                  reads=[rd["VA_all"]], writes=[r_vS[vb]])
            kbs = []
            for mm in range(2):
                m = 2 * h + mm
                kb = (h % 2) * 2 + mm
                kbs.append(kb)
                P.dma(kT[kb][0:64], dr["KTA_all"][:, m].rearrange("r d t -> d r t"), reads=[rd["KTA_all"]], writes=[r_kT[kb]])
                P.dma(qT[kb][0:64], dr["QTA"][m], reads=[rd["QTA"]], writes=[r_qT[kb]])
            for T in range(4):
                for mm in range(2):
                    kb = kbs[mm]
                    accb = 3 + (mm)
                    denb = 5 + (mm)

                    def post(T=T, mm=mm, h=h, accb=accb, denb=denb):
                        P.dve(lambda e: e.reciprocal(rb, self.ps(denb)), [self.rps[denb]], [r_rb])
                        if mm == 1:
                            P.dve(lambda e: e.tensor_scalar(rb, rb, neglam, None, ALU.mult), [r_rb, r_lam], [r_rb])
                        P.dve(lambda e: e.tensor_tensor(om[mm], self.ps(accb), rb, ALU.mult),
                              [self.rps[accb], r_rb], [r_om[mm]])

                        def st2():
                            if mm == 1:
                                P.dve(lambda e: e.tensor_tensor(oa, om[0], om[1], ALU.add), [r_om[0], r_om[1]], [r_oa])
                                P.act(lambda e: e.activation(sqb, oa, AF.Square), [r_oa], [r_sqb])

                                def st3():
                                    P.pe(lambda e: e.matmul(self.ps(7), self.onesb, sqb, start=True, stop=True),
                                         [self.rcb, r_sqb], [self.rps[7]])
                                    P.dve(lambda e: e.tensor_scalar(rsd, self.ps(7), 1.0 / 128, EPS, ALU.mult, ALU.add),
                                          [self.rps[7]], [r_rsd])
                                    P.act(lambda e: e.activation(rsd, rsd, AF.Ln), [r_rsd], [r_rsd])
                                    P.act(lambda e: e.activation(rsd, rsd, AF.Exp, scale=-0.5), [r_rsd], [r_rsd])
                                    ob = (h * 4 + T) % 2
                                    P.dve(lambda e: e.scalar_tensor_tensor(ost[ob], oa, gcol, rsd, ALU.mult, ALU.mult),
                                          [r_oa, r_gcol, r_rsd], [r_ost[ob]])
                                    P.dma(mixT[h * 128:(h + 1) * 128, T * 512:(T + 1) * 512], ost[ob], reads=[r_ost[ob]],
                                          pwrites=[rmix])
                                pending.append([2, st3])
                        pending.append([2, st2])

                    run_map(128, kT[kb], qT[kb], vS[vb], r_kT[kb], r_qT[kb], r_vS[vb], T, accb, denb, False, 0, post)
        flush()
        if STOP == 'A':
            P.barrier()
            return
        if SKIP_AB:
            flush()
            P.barrier()
            self.phase2c(L)
            return
        vSB = [v.rearrange("p r j d -> p (r j d)")[:, 0:2 * 16 * 65].rearrange("p (r j d) -> p r j d", r=2, j=16) for v in vS]
        for h in range(4):
            vb = h % 2
            P.dma(vSB[vb], dr["VB_all"][:, :, h, :].rearrange("r (j p) d -> p r j d", p=128),
                  reads=[rd["VB_all"]], writes=[r_vS[vb]])
            kb = h % NBUF
            P.dma(kT[kb][0:65], dr["KTB_all"][:, h].rearrange("r d t -> d r t"), reads=[rd["KTB_all"]], writes=[r_kT[kb]])
            P.dma(qT[kb][0:64], dr["QTB"][h, 0:64, :], reads=[rd["QTB"]], writes=[r_qT[kb]])
            P.dma(qT[kb][64:65], dr["QTBc"][h:h + 1, :], reads=[rd["QTBc"]], pwrites=[r_qT[kb]])
            for T in range(4):
                accb = 3 + (T % 2)

                def post(T=T, h=h, accb=accb):
                    P.act(lambda e: e.copy(dS[0:1, :], self.ps(accb)[0:1, :]), [self.rps[accb]], [r_dS])
                    P.dve(lambda e: e.reciprocal(dS[0:1, :], dS[0:1, :]), [r_dS], [r_dS])

                    def st2():
                        P.pe(lambda e: e.matmul(self.ps(7)[0:65, :], onesf[0:1, 0:65], dS[0:1, :], start=True, stop=True),
                             [self.rcf, r_dS], [self.rps[7]])
                        P.act(lambda e: e.copy(rb[0:65, :], self.ps(7)[0:65, :]), [self.rps[7]], [r_rb])
                        ob = (h * 4 + T) % 2
                        P.dve(lambda e: e.tensor_tensor(ost[ob][0:65, :], self.ps(accb)[0:65, :], rb[0:65, :], ALU.mult),
                              [self.rps[accb], r_rb], [r_ost[ob]])
                        P.dma(mixT[512 + h * 64:512 + (h + 1) * 64, T * 512:(T + 1) * 512], ost[ob][1:65, :],
                              reads=[r_ost[ob]], pwrites=[rmix])
                    pending.append([3, st2])

                run_map(128, kT[kb], qT[kb], vSB[vb], r_kT[kb], r_qT[kb], r_vS[vb], T, accb, None, True, h, post)
        flush()
        P.barrier()
        if STOP == 'B':
            return
        self.phase2c(L)

    def phase2c(self, L):
        A, P, w, dr, rd = self.A, self.P, self.w, self.dr, self.rd
        A.reset()
        Sown = A.bf16(16 * 256).rearrange("p (i n) -> p i n", i=16); r_Sown = Res()
        mark = A.off
        kvS = A.f32(2 * 16 * 256).rearrange("p (r i n) -> p r i n", r=2, i=16); r_kvS = Res()
        P.dma(kvS[0:64], dr["KV_all"].rearrange("r i d n -> d r i n"), reads=[rd["KV_all"]], writes=[r_kvS])
        Sst = A.f32(32 * 256).rearrange("p (c n) -> p c n", c=32)
        r_Sh = [Res() for _ in range(4)]
        P.dve(lambda e: e.memset(Sst[0:64, 0, :], 0.0), [], r_Sh)
        cdec = [math.exp(128.0 * math.log1p(-2.0 ** (-5.0 - hh))) for hh in range(4)]
        for c in range(31):
            for hh in range(4):
                hs_ = slice(hh * 64, (hh + 1) * 64)
                P.dve(lambda e, c=c, hh=hh, hs_=hs_: e.scalar_tensor_tensor(Sst[0:64, c + 1, hs_], Sst[0:64, c, hs_], cdec[hh],
                                                                           kvS[0:64, c % 2, c // 2, hs_], ALU.mult, ALU.add),
                      [r_Sh[hh], r_kvS], pwrites=[r_Sh[hh]])
        Sv = Sst.rearrange("p (i r) n -> p i r n", r=2)
        St = A.f32(16 * 256).rearrange("p (i n) -> p i n", i=16); r_St = Res()
        gsel = self.cst("gsel")
        P.dve(lambda e: e.tensor_scalar(St[0:64], Sv[0:64, :, 0, :], gsel[0:64, 0:1], None, ALU.mult), r_Sh + [self.rcf], [r_St])
        P.dve(lambda e: e.scalar_tensor_tensor(Sown[0:64], Sv[0:64, :, 1, :], gsel[0:64, 1:2], St[0:64], ALU.mult, ALU.add),
              r_Sh + [self.rcf, r_St], [r_Sown])
        P.barrier()
        A.off = mark
        gng = A.f32(256); r_gng = Res()
        self.bcast_load(gng, w["ret_gn"][L, :], r_gng)
        gcall = A.bf16(16 * 256).rearrange("p (i n) -> p i n", i=16); r_gcall = Res()
        P.dma(gcall, dr["CB"][:, 768:1024].rearrange("(i p) n -> p i n", p=128), reads=[rd["CB"]], writes=[r_gcall])
        NB3 = 3
        NC4 = 4
        cbt = [A.bf16(1024) for _ in range(NC4)]; r_cbt = [Res() for _ in range(NC4)]
        qkT = [A.bf16(8 * 128).rearrange("p (s t) -> p s t", s=8) for _ in range(2)]; r_qkT = [Res(), Res()]
        qd = [A.bf16(4 * 128).rearrange("p (s t) -> p s t", s=4) for _ in range(NB3)]; r_qd = [Res() for _ in range(NB3)]
        PTb = [A.bf16(512).rearrange("p (s t) -> p s t", s=4) for _ in range(2)]; r_PT = [Res(), Res()]
        innerT = self.cst("innerT").rearrange("p (s t) -> p s t", s=4)
        qdecT = self.cst("qdecT", 64).rearrange("p (s t) -> p s t", s=4)
        oall = A.f32(16 * 256); r_oall = Res()
        oall_i = oall.rearrange("p (i n) -> p i n", i=16)
        oall3 = oall.rearrange("p (s n) -> p s n", n=64)
        sq = A.f32(16 * 256); r_sq = Res()
        sq3 = sq.rearrange("p (s n) -> p s n", n=64)
        stm = A.f32(64); r_stm = Res()
        stv = A.f32(64); r_stv = Res()
        ym = A.bf16(16 * 256).rearrange("p (i n) -> p i n", i=16); r_ym = Res()
        yT4 = [A.bf16(1024).rearrange("p (s t) -> p s t", s=8) for _ in range(2)]; r_yT4 = [Res(), Res()]
        mixT, rmix = dr["mixT"], rd["mixT"]
        pb = self.psb(0).rearrange("p (s t) -> p s t", s=8)
        pa = self.ps(1).rearrange("p (s t) -> p s t", s=4)

        def stage1(i):
            c3, b, c4 = i % NB3, i % 2, i % NC4
            for s_ in range(8):
                P.pe(lambda e, s_=s_, c4=c4: e.transpose(pb[0:64, s_, :], cbt[c4][:, s_ * 64:(s_ + 1) * 64], self.identb),
                     [r_cbt[c4], self.rcb], writes=[self.rps[0]] if s_ == 0 else (), pwrites=() if s_ == 0 else [self.rps[0]])
            P.dve(lambda e, b=b: e.tensor_scalar(qkT[b][0:64, 0:4, :], pb[0:64, 0:4, :], 0.125, None, ALU.mult),
                  [self.rps[0]], pwrites=[r_qkT[b]])
            P.dve(lambda e, b=b: e.tensor_copy(qkT[b][0:64, 4:8, :], pb[0:64, 4:8, :]), [self.rps[0]], pwrites=[r_qkT[b]])
            P.dve(lambda e, b=b, c3=c3: e.tensor_tensor(qd[c3][0:64], qkT[b][0:64, 0:4, :], qdecT, ALU.mult),
                  [r_qkT[b], self.rcf], [r_qd[c3]])

        def stage2(i):
            b = i % 2
            for hh in range(4):
                P.pe(lambda e, hh=hh, b=b: e.matmul(pa[:, hh, :], qkT[b][0:64, 4 + hh, :], qkT[b][0:64, hh, :], start=True, stop=True),
                     [r_qkT[b]], writes=[self.rps[1]] if hh == 0 else (), pwrites=() if hh == 0 else [self.rps[1]])
            P.dve(lambda e, b=b: e.tensor_tensor(PTb[b], pa, innerT, ALU.mult), [self.rps[1], self.rcf], [r_PT[b]])

        def stage3(i):
            c3, b, c4 = i % NB3, i % 2, i % NC4
            ob = 2 + b * 5
            po = self.ps(ob)[:, 0:256]
            for hh in range(4):
                P.pe(lambda e, hh=hh, b=b, c4=c4, po=po: e.matmul(po[:, hh * 64:(hh + 1) * 64], PTb[b][:, hh, :],
                                                                  cbt[c4][:, 512 + hh * 64:512 + (hh + 1) * 64], start=True, stop=False),
                     [r_PT[b], r_cbt[c4]], writes=[self.rps[ob]] if hh == 0 else (), pwrites=() if hh == 0 else [self.rps[ob]])
                P.pe(lambda e, hh=hh, c3=c3, i=i, po=po: e.matmul(po[:, hh * 64:(hh + 1) * 64], qd[c3][0:64, hh, :],
                                                                  Sown[0:64, i, hh * 64:(hh + 1) * 64], start=False, stop=True),
                     [r_qd[c3], r_Sown], pwrites=[self.rps[ob]])
            P.dve(lambda e, i=i, po=po: e.tensor_copy(oall_i[:, i, :], po), [self.rps[ob]], pwrites=[r_oall])

        def load(i):
            P.dma(cbt[i % NC4], dr["CB"][i * 128:(i + 1) * 128, :], reads=[rd["CB"]], writes=[r_cbt[i % NC4]])

        load(0)
        load(1)
        for j in range(16 + 2):
            if j < 16:
                stage1(j)
            if 0 <= j - 1 < 16:
                stage2(j - 1)
            if 0 <= j - 2 < 16:
                stage3(j - 2)
            if j + 2 < 16:
                load(j + 2)
        def bc64(ap2):
            return ap2.unsqueeze(2).broadcast_to([128, 64, 64])
        P.dve(lambda e: e.tensor_reduce(stm, oall3, AX.X, ALU.add), [r_oall], [r_stm])
        P.dve(lambda e: e.tensor_scalar(stm, stm, -1.0 / 64, None, ALU.mult), [r_stm], [r_stm])
        P.dve(lambda e: e.tensor_tensor(oall3, oall3, bc64(stm), ALU.add), [r_oall, r_stm], [r_oall])
        P.dve(lambda e: e.tensor_tensor(sq, oall, oall, ALU.mult), [r_oall], [r_sq])
        P.dve(lambda e: e.tensor_reduce(stv, sq3, AX.X, ALU.add), [r_sq], [r_stv])
        P.dve(lambda e: e.tensor_scalar(stv, stv, 1.0 / 64, EPS, ALU.mult, ALU.add), [r_stv], [r_stv])
        P.act(lambda e: e.activation(stv, stv, AF.Ln), [r_stv], [r_stv])
        P.act(lambda e: e.activation(stv, stv, AF.Exp, scale=-0.5), [r_stv], [r_stv])
        P.dve(lambda e: e.tensor_tensor(oall3, oall3, bc64(stv), ALU.mult), [r_oall, r_stv], [r_oall])
        P.dve(lambda e: e.tensor_tensor(oall_i, oall_i, gng.unsqueeze(1).broadcast_to([128, 16, 256]), ALU.mult),
              [r_oall, r_gng], [r_oall])
        gflat = gcall.rearrange("p i n -> p (i n)")
        P.act(lambda e: e.activation(sq, gflat, AF.Exp, scale=-1.0), [r_gcall, r_sq], [r_sq])
        P.dve(lambda e: e.tensor_scalar(sq, sq, 1.0, None, ALU.add), [r_sq], [r_sq])
        P.dve(lambda e: e.reciprocal(sq, sq), [r_sq], [r_sq])
        P.dve(lambda e: e.tensor_tensor(sq, sq, gflat, ALU.mult), [r_sq, r_gcall], [r_sq])
        P.dve(lambda e: e.tensor_tensor(ym.rearrange("p i n -> p (i n)"), oall, sq, ALU.mult), [r_oall, r_sq], [r_ym])
        for q4 in range(4):
            bank = 3 + q4
            pt = self.psb(bank).rearrange("p (s t) -> p s t", s=8)
            for ii in range(4):
                for c in range(2):
                    s_ = ii * 2 + c
                    P.pe(lambda e, s_=s_, ii=ii, c=c, q4=q4, pt=pt: e.transpose(pt[:, s_, :], ym[:, q4 * 4 + ii, c * 128:(c + 1) * 128], self.identb),
                         [r_ym, self.rcb], writes=[self.rps[bank]] if s_ == 0 else (), pwrites=() if s_ == 0 else [self.rps[bank]])
            yb = q4 % 2
            P.dve(lambda e, yb=yb, pt=pt: e.tensor_copy(yT4[yb], pt), [self.rps[bank]], [r_yT4[yb]])
            ysrc = yT4[yb].rearrange("p (ii c) t -> p ii c t", c=2)
            for c in range(2):
                P.dma(mixT[768 + c * 128:768 + (c + 1) * 128, q4 * 512:(q4 + 1) * 512].rearrange("p (ii t) -> p ii t", t=128),
                      ysrc[:, :, c, :], reads=[r_yT4[yb]], pwrites=[rmix])
        P.barrier()

    def phase3(self, L):
        A, P, w, dr, rd = self.A, self.P, self.w, self.dr, self.rd
        A.reset()
        xnT_f = A.bf16(8 * NT).rearrange("p (k t) -> p k t", k=8)
        cw_f = A.f32(16 * 8).rearrange("p (i e) -> p i e", i=16)
        markA = A.off
        mA = A.bf16(4 * NT).rearrange("p (c t) -> p c t", c=4); r_mA = Res()
        mB = A.bf16(4 * NT).rearrange("p (c t) -> p c t", c=4); r_mB = Res()
        mC = A.bf16(2 * NT).rearrange("p (c t) -> p c t", c=2); r_mC = Res()
        mixT = dr["mixT"]
        P.dma(mA, mixT[0:512, :].rearrange("(c p) t -> p c t", p=128), reads=[rd["mixT"]], writes=[r_mA])
        P.dma(mB[0:64], mixT[512:768, :].rearrange("(c p) t -> p c t", p=64), reads=[rd["mixT"]], writes=[r_mB])
        P.dma(mC, mixT[768:1024, :].rearrange("(c p) t -> p c t", p=128), reads=[rd["mixT"]], writes=[r_mC])
        woA = A.bf16(4 * 1024).rearrange("p (c n) -> p c n", c=4); r_woA = Res()
        woB = A.bf16(4 * 1024).rearrange("p (c n) -> p c n", c=4); r_woB = Res()
        woC = A.bf16(2 * 1024).rearrange("p (c n) -> p c n", c=2); r_woC = Res()
        wo = w["w_out"][L]
        self.wload(woA, wo[0:512, :].rearrange("(c p) n -> p c n", p=128), r_woA)
        self.wload(woB[0:64], wo[512:768, :].rearrange("(c p) n -> p c n", p=64), r_woB)
        self.wload(woC, wo[768:1024, :].rearrange("(c p) n -> p c n", p=128), r_woC)
        nb = 0
        for i in range(16):
            tl = slice(i * 128, (i + 1) * 128)
            for half in range(2):
                bank = nb % 4
                nb += 1
                hl = slice(half * 512, (half + 1) * 512)
                ops = [(mA[:, c, tl], woA[:, c, hl], r_mA, r_woA) for c in range(4)]
                ops += [(mB[0:64, c, tl], woB[0:64, c, hl], r_mB, r_woB) for c in range(4)]
                ops += [(mC[:, c, tl], woC[:, c, hl], r_mC, r_woC) for c in range(2)]
                for n, (l_, r_, rl, rr) in enumerate(ops):
                    P.pe(lambda e, l_=l_, r_=r_, n=n, bank=bank: e.matmul(self.ps(bank), l_, r_, start=(n == 0), stop=(n == 9)),
                         [rl, rr], writes=[self.rps[bank]] if n == 0 else (), pwrites=() if n == 0 else [self.rps[bank]])
                P.dve(lambda e, i=i, hl=hl, bank=bank: e.tensor_tensor(self.hs[:, i, hl], self.hs[:, i, hl], self.ps(bank), ALU.add),
                      [self.rhs[i], self.rps[bank]], [self.rhs[i]])
        if "dbg1" in self.dr and L == 0:
            self.store_h("dbg1", is_out=True)
        is_moe = (L % 2 == 1)
        j = L // 2
        gt = A.f32(1024); r_gt = Res()
        self.bcast_load(gt, w["norm_ffn"][L, :], r_gt)
        rstd = A.f32(16); r_rstd = Res()
        scr = A.f32(1024); r_scr = Res()
        self.rms_stats(rstd, r_rstd, scr, r_scr)
        xnT = xnT_f; r_xnT = Res()
        xnb = [A.bf16(1024) for _ in range(2)]; r_xnb = [Res(), Res()]
        cw = cw_f; r_cw = Res()
        mark_scratch = A.off
        if is_moe:
            rtile = A.f32(64).rearrange("p (k e) -> p k e", k=8); r_rtile = Res()
            P.dma(rtile, w["router"][j].rearrange("(k p) e -> p k e", p=128), writes=[r_rtile])
            xnf = A.f32(1024); r_xnf = Res()
            xT32 = [A.f32(1024) for _ in range(2)]; r_xT32 = [Res(), Res()]
            lgA = A.f32(128).rearrange("p (i e) -> p i e", i=16); r_lgA = Res()
            l2 = A.f32(128).rearrange("p (i e) -> p i e", i=16); r_l2 = Res()
            eq1 = A.f32(128).rearrange("p (i e) -> p i e", i=16); r_eq1 = Res()
            eq2 = A.f32(128).rearrange("p (i e) -> p i e", i=16); r_eq2 = Res()
            m1 = A.f32(16); r_m1 = Res()
            m2 = A.f32(16); r_m2 = Res()
            g1 = A.f32(16); r_g1 = Res()
            g2 = A.f32(16); r_g2 = Res()
        for i in range(16):
            if is_moe:
                self.norm_transpose(i, rstd, r_rstd, gt, r_gt, xnb[i % 2], r_xnb[i % 2], xnT, r_xnT, 7, xnf, r_xnf)
                tp_ = i % 2
                bks = (0, 1) if tp_ == 0 else (2, 3)
                for kc in range(8):
                    bk = bks[kc // 4]
                    col = (kc % 4) * 128
                    P.pe(lambda e, kc=kc, bk=bk, col=col: e.transpose(self.ps(bk)[:, col:col + 128], xnf[:, kc * 128:(kc + 1) * 128],
                                                                      self.cst("ident")),
                         [r_xnf, self.rcf], writes=[self.rps[bk]] if kc % 4 == 0 else (), pwrites=() if kc % 4 == 0 else [self.rps[bk]])
                for hf in range(2):
                    P.act(lambda e, hf=hf, tp_=tp_, bks=bks: e.copy(xT32[tp_][:, hf * 512:(hf + 1) * 512], self.ps(bks[hf])),
                          [self.rps[bks[hf]]], writes=[r_xT32[tp_]] if hf == 0 else (), pwrites=() if hf == 0 else [r_xT32[tp_]])
                lb = 4 + tp_
                for kc in range(8):
                    P.pe(lambda e, kc=kc, lb=lb, tp_=tp_: e.matmul(self.ps(lb)[:, 0:8], xT32[tp_][:, kc * 128:(kc + 1) * 128], rtile[:, kc, :],
                                                                  start=(kc == 0), stop=(kc == 7)),
                         [r_xT32[tp_], r_rtile], writes=[self.rps[lb]] if kc == 0 else (), pwrites=() if kc == 0 else [self.rps[lb]])
                P.dve(lambda e, lb=lb, i=i: e.tensor_copy(lgA[:, i, :], self.ps(lb)[:, 0:8]), [self.rps[lb]], pwrites=[r_lgA])
            else:
                self.norm_transpose(i, rstd, r_rstd, gt, r_gt, xnb[i % 2], r_xnb[i % 2], xnT, r_xnT, 7)
        if is_moe:
            def bc(ap2):
                return ap2.unsqueeze(2).broadcast_to([128, 16, 8])
            P.dve(lambda e: e.tensor_reduce(m1, lgA, AX.X, ALU.max), [r_lgA], [r_m1])
            P.dve(lambda e: e.tensor_tensor(eq1, lgA, bc(m1), ALU.is_equal), [r_lgA, r_m1], [r_eq1])
            P.dve(lambda e: e.scalar_tensor_tensor(l2, eq1, -1e30, lgA, ALU.mult, ALU.add), [r_eq1, r_lgA], [r_l2])
            P.dve(lambda e: e.tensor_reduce(m2, l2, AX.X, ALU.max), [r_l2], [r_m2])
            P.dve(lambda e: e.tensor_tensor(eq2, l2, bc(m2), ALU.is_equal), [r_l2, r_m2], [r_eq2])
            P.dve(lambda e: e.tensor_tensor(g2, m2, m1, ALU.subtract), [r_m1, r_m2], [r_g2])
            P.act(lambda e: e.activation(g2, g2, AF.Exp), [r_g2], [r_g2])
            P.dve(lambda e: e.tensor_scalar(g1, g2, 1.0, None, ALU.add), [r_g2], [r_g1])
            P.dve(lambda e: e.reciprocal(g1, g1), [r_g1], [r_g1])
            P.dve(lambda e: e.tensor_tensor(g2, g2, g1, ALU.mult), [r_g2, r_g1], [r_g2])
            P.dve(lambda e: e.tensor_tensor(eq1, eq1, bc(g1), ALU.mult), [r_eq1, r_g1], [r_eq1])
            P.dve(lambda e: e.tensor_tensor(eq2, eq2, bc(g2), ALU.mult), [r_eq2, r_g2], [r_eq2])
            P.dve(lambda e: e.tensor_tensor(cw, eq1, eq2, ALU.add), [r_eq1, r_eq2], [r_cw])
        P.barrier()
        A.off = markA
        if is_moe:
            experts = [(w["moe_w_gate"][j, ex], w["moe_w_up"][j, ex], w["moe_w_down"][j, ex], ex) for ex in range(8)]
            groups = [4] * 7
        else:
            experts = [(w["dense_w_gate"][j], w["dense_w_up"][j], w["dense_w_down"][j], None)]
            groups = [4] * 5 + [2]
        NW = 2
        wg = [A.bf16(8 * 512).rearrange("p (k n) -> p k n", k=8) for _ in range(NW)]; r_wg = [Res() for _ in range(NW)]
        wu = [A.bf16(8 * 512).rearrange("p (k n) -> p k n", k=8) for _ in range(NW)]; r_wu = [Res() for _ in range(NW)]
        wd = [A.bf16(4 * 1024).rearrange("p (c n) -> p c n", c=4) for _ in range(NW)]; r_wd = [Res() for _ in range(NW)]
        HT = [A.bf16(4 * 512).rearrange("p (c t) -> p c t", c=4) for _ in range(2)]; r_HT = [Res(), Res()]
        sg = [A.bf16(512) for _ in range(2)]; r_sg = [Res(), Res()]
        jobs = []
        for (g_, u_, d_, ex) in experts:
            f0 = 0
            for gs in groups:
                jobs.append((g_, u_, d_, ex, f0, gs))
                f0 += gs * 128

        def issue_w(n):
            g_, u_, d_, ex, f0, gs = jobs[n]
            s = n % NW
            fw = gs * 128
            self.wload(wg[s][:, :, 0:fw], g_[:, f0:f0 + fw].rearrange("(k p) n -> p k n", p=128), r_wg[s])
            self.wload(wu[s][:, :, 0:fw], u_[:, f0:f0 + fw].rearrange("(k p) n -> p k n", p=128), r_wu[s])
            self.wload(wd[s][:, 0:gs, :], d_[f0:f0 + fw, :].rearrange("(c p) n -> p c n", p=128), r_wd[s])

        if NJOBS is not None:
            jobs = jobs[:NJOBS]
        issue_w(0)
        hcnt = 0
        pcnt = 0
        prev_down = None
        for n, (g_, u_, d_, ex, f0, gs) in enumerate(jobs):
            s = n % NW
            for T in range(4):
                hb = hcnt % 2
                hcnt += 1
                for c in range(gs):
                    bg, bu = (0, 1) if (pcnt % 2 == 0) else (2, 3)
                    pcnt += 1
                    for (bank, wt, r_wt) in ((bg, wg[s], r_wg[s]), (bu, wu[s], r_wu[s])):
                        for k in range(8):
                            P.pe(lambda e, bank=bank, wt=wt, k=k, c=c, T=T, xnT=xnT: e.matmul(
                                self.ps(bank), wt[:, k, c * 128:(c + 1) * 128], xnT[:, k, T * 512:(T + 1) * 512],
                                start=(k == 0), stop=(k == 7)),
                                [r_wt, r_xnT], writes=[self.rps[bank]] if k == 0 else (),
                                pwrites=() if k == 0 else [self.rps[bank]])
                    sb_ = pcnt % 2
                    P.act(lambda e, bg=bg, sb_=sb_: e.activation(sg[sb_], self.ps(bg), SILU_FN or AF.Silu), [self.rps[bg]], [r_sg[sb_]])
                    P.dve(lambda e, bu=bu, sb_=sb_, hb=hb, c=c: e.tensor_tensor(HT[hb][:, c, :], sg[sb_], self.ps(bu), ALU.mult),
                          [r_sg[sb_], self.rps[bu]], pwrites=[r_HT[hb]])
                def down(T=T, hb=hb, s=s, gs=gs, ex=ex):
                    for sub in range(4):
                        i = T * 4 + sub
                        for half in range(2):
                            bank = 4 + (sub * 2 + half) % 4
                            hl = slice(half * 512, (half + 1) * 512)
                            for c in range(gs):
                                P.pe(lambda e, bank=bank, c=c, sub=sub, hl=hl, hb=hb, s=s, gs=gs: e.matmul(
                                    self.ps(bank), HT[hb][:, c, sub * 128:(sub + 1) * 128], wd[s][:, c, hl],
                                    start=(c == 0), stop=(c == gs - 1)),
                                    [r_HT[hb], r_wd[s]], writes=[self.rps[bank]] if c == 0 else (),
                                    pwrites=() if c == 0 else [self.rps[bank]])
                            if ex is None:
                                P.dve(lambda e, i=i, hl=hl, bank=bank: e.tensor_tensor(self.hs[:, i, hl], self.hs[:, i, hl], self.ps(bank), ALU.add),
                                      [self.rhs[i], self.rps[bank]], [self.rhs[i]])
                            else:
                                P.dve(lambda e, i=i, hl=hl, bank=bank, ex=ex: e.scalar_tensor_tensor(
                                    self.hs[:, i, hl], self.ps(bank), cw[:, i, ex:ex + 1], self.hs[:, i, hl], ALU.mult, ALU.add),
                                    [self.rhs[i], self.rps[bank], r_cw], [self.rhs[i]])
                if prev_down is not None:
                    prev_down()
                prev_down = down
                if T == 0 and n + 1 < len(jobs):
                    issue_w(n + 1)
        if prev_down is not None:
            prev_down()
        if "dbg2" in self.dr and L == 0:
            self.store_h("dbg2", is_out=True)
        P.barrier()
        A.reset()
        gt = A.f32(1024); r_gt = Res()
        self.bcast_load(gt, w["ple_norm"][L, :], r_gt)
        rstd = A.f32(16); r_rstd = Res()
        scr = A.f32(1024); r_scr = Res()
        self.rms_stats(rstd, r_rstd, scr, r_scr)
        pg = A.bf16(8 * 1024).rearrange("p (k n) -> p k n", k=8); r_pg = Res()
        pj = A.bf16(2 * 1024).rearrange("p (k n) -> p k n", k=2); r_pj = Res()
        self.wload(pg, w["ple_gate"][L].rearrange("(k p) n -> p k n", p=128), r_pg)
        self.wload(pj, w["ple_proj"][L].rearrange("(k p) n -> p k n", p=128), r_pj)
        xnT = A.bf16(8 * NT).rearrange("p (k t) -> p k t", k=8); r_xnT = Res()
        xnb = [A.bf16(1024) for _ in range(2)]; r_xnb = [Res(), Res()]
        pb16 = [A.bf16(256) for _ in range(2)]; r_pb16 = [Res(), Res()]
        pT = [A.bf16(256).rearrange("p (k t) -> p k t", k=2) for _ in range(2)]; r_pT = [Res(), Res()]
        sig = [A.f32(1024) for _ in range(2)]; r_sig = [Res(), Res()]
        for i in range(16):
            self.norm_transpose(i, rstd, r_rstd, gt, r_gt, xnb[i % 2], r_xnb[i % 2], xnT, r_xnT, 7)
        ptv = self.psb(6).rearrange("p (k t) -> p k t", k=8)

        def prep(i):
            b = i % 2
            self.wload(pb16[b], w["p"][L, i * 128:(i + 1) * 128, :], r_pb16[b])
            for k in range(2):
                P.pe(lambda e, k=k, b=b: e.transpose(ptv[:, k, :], pb16[b][:, k * 128:(k + 1) * 128], self.identb),
                     [r_pb16[b], self.rcb], writes=[self.rps[6]] if k == 0 else (), pwrites=() if k == 0 else [self.rps[6]])
            P.dve(lambda e, b=b: e.tensor_copy(pT[b], ptv[:, 0:2, :]), [self.rps[6]], [r_pT[b]])

        prep(0)
        for i in range(16):
            b = i % 2
            tl = slice(i * 128, (i + 1) * 128)
            if i + 1 < 16:
                prep(i + 1)
            for half in range(2):
                hl = slice(half * 512, (half + 1) * 512)
                bgate = half + 2 * (i % 2)
                bpp = 4 + half
                for k in range(8):
                    P.pe(lambda e, k=k, tl=tl, hl=hl, bgate=bgate: e.matmul(self.ps(bgate), xnT[:, k, tl], pg[:, k, hl],
                                                                          start=(k == 0), stop=(k == 7)),
                         [r_xnT, r_pg], writes=[self.rps[bgate]] if k == 0 else (), pwrites=() if k == 0 else [self.rps[bgate]])
                for k in range(2):
                    P.pe(lambda e, k=k, b=b, hl=hl, bpp=bpp: e.matmul(self.ps(bpp), pT[b][:, k, :], pj[:, k, hl],
                                                                      start=(k == 0), stop=(k == 1)),
                         [r_pT[b], r_pj], writes=[self.rps[bpp]] if k == 0 else (), pwrites=() if k == 0 else [self.rps[bpp]])
                P.act(lambda e, b=b, hl=hl, bgate=bgate: e.activation(sig[b][:, hl], self.ps(bgate), AF.Sigmoid),
                      [self.rps[bgate]], pwrites=[r_sig[b]])
                P.dve(lambda e, b=b, hl=hl, bpp=bpp: e.tensor_tensor(sig[b][:, hl], sig[b][:, hl], self.ps(bpp), ALU.mult),
                      [r_sig[b], self.rps[bpp]], pwrites=[r_sig[b]])
                P.dve(lambda e, b=b, hl=hl, i=i: e.tensor_tensor(self.hs[:, i, hl], self.hs[:, i, hl], sig[b][:, hl], ALU.add),
                      [r_sig[b], self.rhs[i]], [self.rhs[i]])
        P.barrier()

    def final_norm(self):
        A, P, w, dr, rd = self.A, self.P, self.w, self.dr, self.rd
        A.reset()
        gt = A.f32(1024); r_gt = Res()
        self.bcast_load(gt, w["final_norm"], r_gt)
        rstd = A.f32(16); r_rstd = Res()
        scr = A.f32(1024); r_scr = Res()
        self.rms_stats(rstd, r_rstd, scr, r_scr)
        ot = [A.f32(1024) for _ in range(2)]; r_ot = [Res(), Res()]
        for i in range(16):
            b = i % 2
            P.dve(lambda e, i=i, b=b: e.scalar_tensor_tensor(ot[b], self.hs[:, i, :], rstd[:, i:i + 1], gt, ALU.mult, ALU.mult),
                  [self.rhs[i], r_rstd, r_gt], [r_ot[b]])
            P.dma(dr["out"][i * 128:(i + 1) * 128, :], ot[b], reads=[r_ot[b]], pwrites=[rd["out"]], is_out=True)

    def coll_one(self, n):
        dr, rd, P = self.dr, self.rd, self.P
        pats = {"KTA": "m d t -> (m d) t", "KTB": "m d t -> (m d) t", "VA": None, "VB": "t h d -> t (h d)",
                "LF": "p i h -> p (i h)", "KV": "i d n -> (i d) n"}
        pata = {"KTA": "r m d t -> (r m d) t", "KTB": "r m d t -> (r m d) t", "VA": "r t d -> (r t) d",
                "VB": "r t h d -> (r t) (h d)", "LF": "r p i h -> (r p) (i h)", "KV": "r i d n -> (r i d) n"}
        src = dr[n + "_own"]
        if pats[n]:
            src = src.rearrange(pats[n])
        dst = dr[n + "_all"].rearrange(pata[n])
        P.coll(dst, src, reads=[rd[n + "_own"]], writes=[rd[n + "_all"]])

    def gather(self):
        dr, rd, P = self.dr, self.rd, self.P
        pats = {"KTA": "m d t -> (m d) t", "KTB": "m d t -> (m d) t", "VA": None, "VB": "t h d -> t (h d)",
                "LF": "p i h -> p (i h)", "KV": "i d n -> (i d) n"}
        pata = {"KTA": "r m d t -> (r m d) t", "KTB": "r m d t -> (r m d) t", "VA": "r t d -> (r t) d",
                "VB": "r t h d -> (r t) (h d)", "LF": "r p i h -> (r p) (i h)", "KV": "r i d n -> (r i d) n"}
        for n in OWN:
            src = dr[n + "_own"]
            if pats[n]:
                src = src.rearrange(pats[n])
            dst = dr[n + "_all"].rearrange(pata[n])
            P.coll(dst, src, reads=[rd[n + "_own"]], writes=[rd[n + "_all"]])
        P.barrier()

    def build(self):
        st = self.stage
        if st == "all":
            self.load_h("x")
            for L in range(2):
                self.phase1(L)
                self.phase2(L)
                self.phase3(L)
            self.final_norm()
        if st == 0:
            self.load_h("x")
            self.phase1(0)
        elif st == 1:
            self.load_h("x")
            self.phase2(0)
            if STOP is None:
                self.phase3(0)
                self.phase1(1, sfx="2")
            self.store_h("hb", is_out=True)
        elif st == 2:
            self.load_h("hb")
            self.phase2(1)
            self.phase3(1)
            self.final_norm()
        for q in self.P.q.values():
            for op in q:
                if op.is_dma and op not in self.P.out_dmas:
                    self.P.out_dmas.append(op)
        self.P.emit()
        return self.nc


_PROGS = {}


def _prog(stage):
    if stage not in _PROGS:
        mkb = MK(stage)
        mkb.build()
        _PROGS[stage] = mkb
    return _PROGS[stage]


def _own_rows(a, g):
    s = a.shape
    return np.ascontiguousarray(a.reshape((16, 2, 128) + s[1:])[:, g].reshape((2048,) + s[1:]))


def _run(stage, per_core):
    mkb = _prog(stage)
    names = list(mkb.w.keys()) + [n for n in mkb.ext_in]
    in_maps = [{n: pc[n] for n in names} for pc in per_core]
    res = run_bass_kernel_spmd(mkb.nc, in_maps[:NCORES], core_ids=list(range(NCORES)))
    return res.results


def kernel(**inputs):
    x = np.asarray(inputs["x"], np.float32)
    p = np.asarray(inputs["p"], np.float32)
    base = []
    for c in range(8):
        b, g = c // 2, c % 2
        cst, cs = make_consts(g)
        d = {k: np.ascontiguousarray(np.asarray(v, np.float32)) for k, v in inputs.items() if k not in ("x", "p")}
        d["cst"] = cst
        d["cs"] = cs
        d["x"] = _own_rows(x[b], g)
        d["p"] = np.stack([_own_rows(p[l, b], g) for l in range(2)])
        base.append(d)

    def relay(results):
        outs = []
        for c in range(8):
            d = dict(base[c])
            pair = [results[2 * (c // 2)], results[2 * (c // 2) + 1]]
            for n in OWN:
                d[n + "_all"] = np.stack([np.asarray(pair[0][n + "_own"]), np.asarray(pair[1][n + "_own"])])
            outs.append(d)
        return outs

    if FUSED:
        r = _run("all", base)
        out = np.zeros((4, 4096, 1024), np.float32)
        for c in range(8):
            b, g = c // 2, c % 2
            out[b].reshape(16, 2, 128, 1024)[:, g] = np.asarray(r[c]["out"]).reshape(16, 128, 1024)
        return out
    r0 = _run(0, base)
    in1 = relay(r0)
    for c in range(8):
        for n in LOCAL:
            in1[c][n] = np.asarray(r0[c][n])
    r1 = _run(1, in1)
    in2 = relay(r1)
    for c in range(8):
        for n in LOCAL:
            in2[c][n] = np.asarray(r1[c][n + "2"])
        in2[c]["hb"] = np.asarray(r1[c]["hb"])
    r2 = _run(2, in2)
    out = np.zeros((4, 4096, 1024), np.float32)
    for c in range(8):
        b, g = c // 2, c % 2
        out[b].reshape(16, 2, 128, 1024)[:, g] = np.asarray(r2[c]["out"]).reshape(16, 128, 1024)
    return out
```
